# Optimizing a Trainium2 kernel written in Bass

```python
import math
import jax, jax.numpy as jnp
from jax import lax
import numpy as np

D_MODEL = 2048
BATCH = 4
SEQ = 4096
DEPTH = 4

GRID_W = 64
CTX_LEN = 256
N_MIXERS = 2
N_DN_LAYERS = (DEPTH + 1) // 2
N_CV_LAYERS = DEPTH // 2
DN_QK_HEADS = 16
DN_V_HEADS = 32
DN_HEAD_K = 128
DN_HEAD_V = 128
DN_QK_DIM = DN_QK_HEADS * DN_HEAD_K
DN_V_DIM = DN_V_HEADS * DN_HEAD_V
DN_CONV_DIM = 2 * DN_QK_DIM + DN_V_DIM
DN_N_DIR = 2
DN_IN_DIM = DN_CONV_DIM + DN_V_DIM + DN_N_DIR * 2 * DN_V_HEADS
DN_CHUNK = 64
SHORT_CONV = 5
CV_INNER = D_MODEL
CV_KERNEL = 31
N_GROUPS = 8
EXPERTS_PER_GROUP = 8
N_EXPERTS = N_GROUPS * EXPERTS_PER_GROUP
TOP_K_IN_GROUP = 2
D_EXPERT = 384
MOE_BLOCK = 128
EPS = 1e-6

kernel_name = "hybrid_deltanet_conformer_hmoe_dit"


def rmsnorm(x, g):
    xf = x.astype(jnp.float32)
    y = xf * lax.rsqrt(jnp.mean(xf * xf, axis=-1, keepdims=True) + EPS)
    return y.astype(x.dtype) * g


def layernorm(x, g, b):
    xf = x.astype(jnp.float32)
    mu = jnp.mean(xf, axis=-1, keepdims=True)
    var = jnp.mean(jnp.square(xf - mu), axis=-1, keepdims=True)
    return ((xf - mu) * lax.rsqrt(var + EPS)).astype(x.dtype) * g + b


def l2norm(x):
    return x * lax.rsqrt(jnp.sum(x * x, axis=-1, keepdims=True) + EPS)


def dwconv_centred(x, w):
    k, ch = w.shape
    return lax.conv_general_dilated(
        x, w[:, None, :], window_strides=(1,), padding=[(k // 2, k // 2)],
        dimension_numbers=("NWC", "WIO", "NWC"), feature_group_count=ch)


def to_column_major(h):
    b, n, d = h.shape
    rows = n // GRID_W
    return h.reshape(b, rows, GRID_W, d).transpose(0, 2, 1, 3).reshape(b, n, d)


def from_column_major(h):
    b, n, d = h.shape
    rows = n // GRID_W
    return h.reshape(b, GRID_W, rows, d).transpose(0, 2, 1, 3).reshape(b, n, d)


def chunk_gated_delta(q, k, v, g, beta, s0):
    b_, h_, n_tok, dk = q.shape
    dv = v.shape[-1]
    cs = DN_CHUNK
    nc = n_tok // cs
    q = (q * dk ** -0.5).reshape(b_, h_, nc, cs, dk)
    k = k.reshape(b_, h_, nc, cs, dk)
    v = v.reshape(b_, h_, nc, cs, dv)
    beta = beta.reshape(b_, h_, nc, cs, 1)
    gc = jnp.cumsum(g.reshape(b_, h_, nc, cs), axis=-1)
    incl = jnp.tril(jnp.ones((cs, cs), bool))
    strict = jnp.tril(jnp.ones((cs, cs), bool), -1)
    decay = jnp.exp(jnp.where(incl, gc[..., :, None] - gc[..., None, :], -jnp.inf))
    kb = k * beta
    m = jnp.where(strict, jnp.einsum("bhncd,bhnsd->bhncs", kb, k) * decay, 0.0)
    a_mat = m + jnp.eye(cs, dtype=m.dtype)
    rhs = jnp.concatenate([v * beta, kb * jnp.exp(gc)[..., None]], axis=-1)
    sol = lax.linalg.triangular_solve(a_mat, rhs, left_side=True, lower=True)
    u, w = sol[..., :dv], sol[..., dv:]
    qk = jnp.einsum("bhncd,bhnsd->bhncs", q, k) * decay
    q_dec = q * jnp.exp(gc)[..., None]
    k_dec = k * jnp.exp(gc[..., -1:] - gc)[..., None]
    g_last = jnp.exp(gc[..., -1])

    def step(s, inp):
        qk_i, q_dec_i, k_dec_i, u_i, w_i, gl_i = inp
        v_new = u_i - jnp.einsum("bhcd,bhde->bhce", w_i, s)
        o_i = jnp.einsum("bhcd,bhde->bhce", q_dec_i, s) + jnp.einsum("bhcs,bhse->bhce", qk_i, v_new)
        s = s * gl_i[..., None, None] + jnp.einsum("bhcd,bhce->bhde", k_dec_i, v_new)
        return s, o_i

    xs = tuple(jnp.moveaxis(t, 2, 0) for t in (qk, q_dec, k_dec, u, w, g_last))
    s_fin, o = lax.scan(step, s0, xs)
    o = jnp.moveaxis(o, 0, 2).reshape(b_, h_, n_tok, dv)
    return o, s_fin


def gated_deltanet(hc, hl, w_in, conv_w, a_log, dt_bias, onorm_g, w_out):
    bsz, lc, _ = hc.shape
    n_tok = lc + hl.shape[1]
    proj = jnp.einsum("btd,de->bte", jnp.concatenate([hc, hl], axis=1), w_in)
    qkv = proj[..., :DN_CONV_DIM]
    z = proj[..., DN_CONV_DIM:DN_CONV_DIM + DN_V_DIM]
    ab = proj[..., DN_CONV_DIM + DN_V_DIM:].astype(jnp.float32).reshape(bsz, n_tok, DN_N_DIR, 2, DN_V_HEADS)
    qkv = jax.nn.silu(jnp.concatenate(
        [dwconv_centred(qkv[:, :lc], conv_w), dwconv_centred(qkv[:, lc:], conv_w)], axis=1)).astype(jnp.float32)
    rep = DN_V_HEADS // DN_QK_HEADS
    q = l2norm(qkv[..., :DN_QK_DIM].reshape(bsz, n_tok, DN_QK_HEADS, DN_HEAD_K))
    k = l2norm(qkv[..., DN_QK_DIM:2 * DN_QK_DIM].reshape(bsz, n_tok, DN_QK_HEADS, DN_HEAD_K))
    q = jnp.repeat(q, rep, axis=2).transpose(0, 2, 1, 3)
    k = jnp.repeat(k, rep, axis=2).transpose(0, 2, 1, 3)
    v = qkv[..., 2 * DN_QK_DIM:].reshape(bsz, n_tok, DN_V_HEADS, DN_HEAD_V).transpose(0, 2, 1, 3)
    g = -jnp.exp(a_log.astype(jnp.float32)) * jax.nn.softplus(ab[:, :, :, 0, :] + dt_bias.astype(jnp.float32))
    beta = jax.nn.sigmoid(ab[:, :, :, 1, :])
    g = jnp.transpose(g, (0, 2, 3, 1))
    beta = jnp.transpose(beta, (0, 2, 3, 1))
    s0 = jnp.zeros((bsz, DN_V_HEADS, DN_HEAD_K, DN_HEAD_V), jnp.float32)
    rev = lambda t: jnp.flip(t, axis=2)
    qc, ql = q[:, :, :lc], q[:, :, lc:]
    kc, kl = k[:, :, :lc], k[:, :, lc:]
    vc, vl = v[:, :, :lc], v[:, :, lc:]
    gf, bf = g[:, 0], beta[:, 0]
    oc_f, sc_f = chunk_gated_delta(qc, kc, vc, gf[..., :lc], bf[..., :lc], s0)
    ol_f, _ = chunk_gated_delta(ql, kl, vl, gf[..., lc:], bf[..., lc:], sc_f)
    gb, bb = g[:, 1], beta[:, 1]
    oc_b, sc_b = chunk_gated_delta(rev(qc), rev(kc), rev(vc), rev(gb[..., :lc]), rev(bb[..., :lc]), s0)
    ol_b, _ = chunk_gated_delta(rev(ql), rev(kl), rev(vl), rev(gb[..., lc:]), rev(bb[..., lc:]), sc_b)
    o = jnp.concatenate([oc_f + rev(oc_b), ol_f + rev(ol_b)], axis=2)
    o = jnp.transpose(o, (0, 2, 1, 3))
    o = rmsnorm(o, onorm_g.astype(jnp.float32)) * jax.nn.silu(
        z.astype(jnp.float32).reshape(bsz, n_tok, DN_V_HEADS, DN_HEAD_V))
    y = jnp.einsum("bte,ed->btd", o.reshape(bsz, n_tok, DN_V_DIM).astype(hc.dtype), w_out)
    return y[:, :lc], y[:, lc:]


def conformer_conv(h, w1, b1, dw, dwb, ln_g, ln_b, w2, b2):
    u = jnp.einsum("btd,de->bte", h, w1) + b1
    a, gate = jnp.split(u, 2, axis=-1)
    u = a * jax.nn.sigmoid(gate)
    u = dwconv_centred(u, dw) + dwb
    u = jax.nn.silu(layernorm(u, ln_g, ln_b))
    return jnp.einsum("bte,ed->btd", u, w2) + b2


def expert_dispatch(h, expert_ids, weights, w_gu, w_down):
    n_t, d = h.shape
    n_assign = n_t * TOP_K_IN_GROUP
    e_flat = expert_ids.reshape(-1)
    tok_flat = jnp.repeat(jnp.arange(n_t, dtype=jnp.int32), TOP_K_IN_GROUP)
    order = jnp.argsort(e_flat)
    e_sorted = e_flat[order]
    counts = jnp.zeros((N_EXPERTS,), jnp.int32).at[e_flat].add(1)
    padded = (counts + MOE_BLOCK - 1) // MOE_BLOCK * MOE_BLOCK
    starts = jnp.cumsum(counts) - counts
    pad_ends = jnp.cumsum(padded)
    pad_starts = pad_ends - padded
    slot = pad_starts[e_sorted] + jnp.arange(n_assign, dtype=jnp.int32) - starts[e_sorted]
    n_blocks = -(-n_assign // MOE_BLOCK) + N_EXPERTS
    n_slots = n_blocks * MOE_BLOCK
    slot_tok = jnp.full((n_slots,), n_t, jnp.int32).at[slot].set(tok_flat[order])
    slot_w = jnp.zeros((n_slots,), h.dtype).at[slot].set(weights.reshape(-1)[order])
    block_expert = jnp.minimum(
        jnp.searchsorted(pad_ends, jnp.arange(n_blocks, dtype=jnp.int32) * MOE_BLOCK, side="right"),
        N_EXPERTS - 1)
    h_pad = jnp.concatenate([h, jnp.zeros((1, d), h.dtype)], axis=0)
    xb = h_pad[slot_tok].reshape(n_blocks, MOE_BLOCK, d)

    def expert_block(args):
        xblk, e = args
        gate, up = jnp.split(xblk @ w_gu[e], 2, axis=-1)
        return (jax.nn.silu(gate) * up) @ w_down[e]

    yb = lax.map(expert_block, (xb, block_expert)).reshape(n_slots, d)
    y = jnp.zeros((n_t + 1, d), h.dtype).at[slot_tok].add(yb * slot_w[:, None])
    return y[:n_t]


def hier_moe(h, w_grp, b_grp, w_exp, b_exp, w_gu, w_down):
    n_t = h.shape[0]
    grp_prob = jax.nn.softmax((h @ w_grp).astype(jnp.float32) + b_grp.astype(jnp.float32), axis=-1)
    grp_p, grp_idx = lax.top_k(grp_prob, 1)
    exp_logits = ((h @ w_exp).astype(jnp.float32) + b_exp.astype(jnp.float32)).reshape(
        n_t, N_GROUPS, EXPERTS_PER_GROUP)
    in_grp = exp_logits[jnp.arange(n_t), grp_idx[:, 0]]
    top_p, top_i = lax.top_k(jax.nn.softmax(in_grp, axis=-1), TOP_K_IN_GROUP)
    weights = grp_p * top_p / jnp.sum(top_p, axis=-1, keepdims=True)
    expert_ids = grp_idx * EXPERTS_PER_GROUP + top_i
    return expert_dispatch(h, expert_ids, weights.astype(h.dtype), w_gu, w_down)


def setup_inputs(seed: int = 0) -> dict:
    key = jax.random.key(seed)
    ks = jax.random.split(key, 40)
    D = D_MODEL
    f32 = jnp.float32
    nrm = lambda k, shape, s: jax.random.normal(k, shape, f32) * s
    dt = jnp.exp(jax.random.uniform(ks[11], (N_DN_LAYERS, DN_N_DIR, DN_V_HEADS), f32,
                                    math.log(1e-3), math.log(1e-1)))
    return {
        "x": nrm(ks[0], (BATCH, SEQ, D), 1.0),
        "c": nrm(ks[1], (BATCH, D), 1.0),
        "ctx": nrm(ks[2], (BATCH, CTX_LEN, D), 1.0),
        "c_ctx": nrm(ks[3], (D,), 1.0),
        "ada_w": nrm(ks[4], (DEPTH, D, 6 * D), 0.5 * D ** -0.5),
        "ada_b": nrm(ks[5], (DEPTH, 6 * D), 0.02),
        "norm1_g": 1.0 + nrm(ks[6], (DEPTH, D), 0.05),
        "norm2_g": 1.0 + nrm(ks[7], (DEPTH, D), 0.05),
        "dn_w_in": nrm(ks[8], (N_DN_LAYERS, D, DN_IN_DIM), D ** -0.5),
        "dn_conv_w": nrm(ks[9], (N_DN_LAYERS, SHORT_CONV, DN_CONV_DIM), SHORT_CONV ** -0.5),
        "dn_a_log": jnp.log(jax.random.uniform(ks[10], (N_DN_LAYERS, DN_N_DIR, DN_V_HEADS), f32, 1.0, 16.0)),
        "dn_dt_bias": dt + jnp.log(-jnp.expm1(-dt)),
        "dn_onorm_g": 1.0 + nrm(ks[12], (N_DN_LAYERS, DN_HEAD_V), 0.05),
        "dn_w_out": nrm(ks[13], (N_DN_LAYERS, DN_V_DIM, D), DN_V_DIM ** -0.5),
        "cv_w1": nrm(ks[14], (N_CV_LAYERS, D, 2 * CV_INNER), D ** -0.5),
        "cv_b1": nrm(ks[15], (N_CV_LAYERS, 2 * CV_INNER), 0.02),
        "cv_dw": nrm(ks[16], (N_CV_LAYERS, CV_KERNEL, CV_INNER), CV_KERNEL ** -0.5),
        "cv_dwb": nrm(ks[17], (N_CV_LAYERS, CV_INNER), 0.02),
        "cv_ln_g": 1.0 + nrm(ks[18], (N_CV_LAYERS, CV_INNER), 0.05),
        "cv_ln_b": nrm(ks[19], (N_CV_LAYERS, CV_INNER), 0.02),
        "cv_w2": nrm(ks[20], (N_CV_LAYERS, CV_INNER, D), CV_INNER ** -0.5),
        "cv_b2": nrm(ks[21], (N_CV_LAYERS, D), 0.02),
        "moe_w_grp": nrm(ks[22], (DEPTH, D, N_GROUPS), D ** -0.5),
        "moe_b_grp": nrm(ks[23], (DEPTH, N_GROUPS), 0.01),
        "moe_w_exp": nrm(ks[24], (DEPTH, D, N_EXPERTS), D ** -0.5),
        "moe_b_exp": nrm(ks[25], (DEPTH, N_EXPERTS), 0.01),
        "moe_w_gu": nrm(ks[26], (DEPTH, N_EXPERTS, D, 2 * D_EXPERT), D ** -0.5),
        "moe_w_down": nrm(ks[27], (DEPTH, N_EXPERTS, D_EXPERT, D), D_EXPERT ** -0.5),
        "final_g": 1.0 + nrm(ks[28], (D,), 0.05),
    }


def reference(x, c, ctx, c_ctx, ada_w, ada_b, norm1_g, norm2_g, dn_w_in, dn_conv_w, dn_a_log,
              dn_dt_bias, dn_onorm_g, dn_w_out, cv_w1, cv_b1, cv_dw, cv_dwb, cv_ln_g, cv_ln_b,
              cv_w2, cv_b2, moe_w_grp, moe_b_grp, moe_w_exp, moe_b_exp, moe_w_gu, moe_w_down, final_g):
    bsz, n_lat, d = x.shape
    lc = ctx.shape[1]
    xl, xc = x, ctx
    for i in range(DEPTH):
        last = i == DEPTH - 1
        j = i // N_MIXERS
        mod_l = (jax.nn.silu(c) @ ada_w[i] + ada_b[i])[:, None, :]
        mod_c = jax.nn.silu(c_ctx) @ ada_w[i] + ada_b[i]
        sh1, sc1, gt1, sh2, sc2, gt2 = jnp.split(mod_l, 6, axis=-1)
        csh1, csc1, cgt1, csh2, csc2, cgt2 = jnp.split(mod_c, 6, axis=-1)
        hl = rmsnorm(xl, norm1_g[i]) * (1.0 + sc1) + sh1
        hc = rmsnorm(xc, norm1_g[i]) * (1.0 + csc1) + csh1
        col_major = (i // N_MIXERS) % 2 == 1
        if col_major:
            hl = to_column_major(hl)
        if i % N_MIXERS == 0:
            yc, yl = gated_deltanet(hc, hl, dn_w_in[j], dn_conv_w[j], dn_a_log[j], dn_dt_bias[j],
                                    dn_onorm_g[j], dn_w_out[j])
        else:
            cv = (cv_w1[j], cv_b1[j], cv_dw[j], cv_dwb[j], cv_ln_g[j], cv_ln_b[j], cv_w2[j], cv_b2[j])
            yl = conformer_conv(hl, *cv)
            yc = None if last else conformer_conv(hc, *cv)
        if col_major:
            yl = from_column_major(yl)
        xl = xl + gt1 * yl
        moe = (moe_w_grp[i], moe_b_grp[i], moe_w_exp[i], moe_b_exp[i], moe_w_gu[i], moe_w_down[i])
        hl2 = rmsnorm(xl, norm2_g[i]) * (1.0 + sc2) + sh2
        if last:
            xl = xl + gt2 * hier_moe(hl2.reshape(bsz * n_lat, d), *moe).reshape(bsz, n_lat, d)
        else:
            xc = xc + cgt1 * yc
            hc2 = rmsnorm(xc, norm2_g[i]) * (1.0 + csc2) + csh2
            y = hier_moe(jnp.concatenate([hc2.reshape(bsz * lc, d), hl2.reshape(bsz * n_lat, d)], axis=0), *moe)
            xc = xc + cgt2 * y[:bsz * lc].reshape(bsz, lc, d)
            xl = xl + gt2 * y[bsz * lc:].reshape(bsz, n_lat, d)
    return rmsnorm(xl, final_g)
```

```python
import time as _time
import numpy as np
from contextlib import ExitStack
import concourse.bass as bass
import concourse.mybir as mybir
from concourse.bass_utils import run_bass_kernel_spmd

F32 = mybir.dt.float32
BF16 = mybir.dt.bfloat16
I32 = mybir.dt.int32
AF = mybir.ActivationFunctionType
ALU = mybir.AluOpType
AX = mybir.AxisListType
ENGS = ["pe", "act", "dve", "pool", "sp"]


class Buf:
    __slots__ = ("name", "lw", "rd")

    def __init__(self, name):
        self.name = name
        self.lw = {}
        self.rd = {}


class Prog:
    def __init__(self):
        self.nc = bass.Bass("TRN2", target_bir_lowering=False)
        self.st = ExitStack()
        self.q = {e: [] for e in ENGS}
        self.cnt = {}
        self.sems = {}
        self.seen = {e: {} for e in ENGS}
        self.nbuf = 0
        self.dsem_pool = {}

    def dram_in(self, name, shape, dt=F32):
        return self.nc.dram_tensor(name, list(shape), dt, kind="ExternalInput").ap()

    def dram_out(self, name, shape, dt=F32):
        return self.nc.dram_tensor(name, list(shape), dt, kind="ExternalOutput").ap()

    def dram(self, name, shape, dt=F32):
        return self.nc.dram_tensor(name, list(shape), dt).ap()

    def sbuf(self, name, shape, dt=F32):
        return self.st.enter_context(self.nc.sbuf_tensor(name, list(shape), dt))

    def psum(self, name, shape, dt=F32):
        return self.st.enter_context(self.nc.psum_tensor(name, list(shape), dt))

    def buf(self, name=None):
        self.nbuf += 1
        return Buf(name or ("b%d" % self.nbuf))

    def _sem(self, key):
        if key not in self.sems:
            self.sems[key] = self.st.enter_context(self.nc.semaphore("s_" + key))
            self.cnt[key] = 0
        return self.sems[key]

    def _deps(self, eng, reads, writes, mykey):
        need = {}

        def add(k, c):
            if need.get(k, 0) < c:
                need[k] = c

        for b in reads:
            for k, c in b.lw.items():
                if k == mykey and eng == "pe":
                    continue
                add(k, c)
        for b in writes:
            for k, c in b.lw.items():
                if k == mykey:
                    continue
                add(k, c)
            for k, c in b.rd.items():
                if k == mykey and k.startswith("e_"):
                    continue
                add(k, c)
        seen = self.seen[eng]
        waits = []
        for k, c in need.items():
            if seen.get(k, 0) >= c:
                continue
            seen[k] = c
            waits.append((k, c))
        return waits

    def _commit(self, key, c, reads, writes):
        for b in reads:
            if b.rd.get(key, 0) < c:
                b.rd[key] = c
        for b in writes:
            b.lw = {key: c}
            b.rd = {}

    def op(self, eng, fn, reads=(), writes=()):
        key = "e_" + eng
        self._sem(key)
        waits = self._deps(eng, reads, writes, key)
        self.cnt[key] += 1
        c = self.cnt[key]
        self.q[eng].append((waits, fn, key, 1))
        self._commit(key, c, reads, writes)

    def dma(self, eng, out_ap, in_ap, reads, writes, owner, **kw):
        key = "d_" + owner.name
        self._sem(key)
        waits = self._deps(eng, reads, writes, key)
        self.cnt[key] += 16
        c = self.cnt[key]
        self.q[eng].append((waits, (lambda e: e.dma_start(out=out_ap, in_=in_ap, **kw)), key, 16))
        self._commit(key, c, reads, writes)

    def mm(self, out, lhsT, rhs, start, stop, reads, writes):
        self.op("pe", lambda e: e.matmul(out, lhsT, rhs, start=start, stop=stop), reads, writes)

    def tr(self, out, in_, ident, reads, writes):
        self.op("pe", lambda e: e.transpose(out, in_, ident), reads, writes)

    def act(self, out, in_, func, reads, writes, eng="act", **kw):
        self.op(eng, lambda e: e.activation(out=out, in_=in_, func=func, **kw), reads, writes)

    def emit(self):
        nc = self.nc
        fin = [(k, c) for k, c in self.cnt.items() if k.startswith("d_") and c > 0]
        engmap = {}
        with nc.Block() as block:
            def mk(name):
                def body(e):
                    for waits, fn, key, inc in self.q[name]:
                        for k, c in waits:
                            e.wait_ge(self.sems[k], c)
                        fn(e).then_inc(self.sems[key], inc)
                    if name == "sp":
                        for k, c in fin:
                            e.wait_ge(self.sems[k], c)
                return body
            block.tensor(mk("pe"))
            block.scalar(mk("act"))
            block.vector(mk("dve"))
            block.gpsimd(mk("pool"))
            block.sync(mk("sp"))
        self.st.close()
        return nc

    def ninstr(self):
        return {e: len(self.q[e]) for e in ENGS}


def run(nc, in_maps, trace=False):
    res = run_bass_kernel_spmd(nc, in_maps, core_ids=list(range(len(in_maps))), trace=trace)
    return res


def _mk(P):
    pass


def tt(P, eng, out, in0, in1, op, R, W):
    P.op(eng, lambda e: e.tensor_tensor(out=out, in0=in0, in1=in1, op=op), R, W)


def ts(P, eng, out, in0, s1, s2, op0, op1, R, W):
    if s2 is None:
        P.op(eng, lambda e: e.tensor_scalar(out=out, in0=in0, scalar1=s1, scalar2=None, op0=op0), R, W)
    else:
        P.op(eng, lambda e: e.tensor_scalar(out=out, in0=in0, scalar1=s1, scalar2=s2, op0=op0, op1=op1), R, W)


def stt(P, eng, out, in0, scalar, in1, op0, op1, R, W):
    P.op(eng, lambda e: e.scalar_tensor_tensor(out=out, in0=in0, scalar=scalar, in1=in1, op0=op0, op1=op1), R, W)


def cp(P, eng, out, in_, R, W):
    if eng == "act":
        P.op(eng, lambda e: e.activation(out=out, in_=in_, func=AF.Copy), R, W)
    else:
        P.op(eng, lambda e: e.tensor_copy(out=out, in_=in_), R, W)


def actf(P, out, in_, func, R, W, **kw):
    P.op("act", lambda e: e.activation(out=out, in_=in_, func=func, **kw), R, W)


EPS = 1e-6


def L(f):
    return f


def consts(P):
    if hasattr(P, "ident"):
        return
    P.ident = P.sbuf("ident", [128, 128], F32)
    P.identb = P.sbuf("identb", [128, 128], BF16)
    P.identB = P.buf("ident")
    ones = P.sbuf("c_ones", [128, 128], F32)
    P.ones = ones
    P.op("pool", lambda e: e.memset(ones[:], 1.0), [], [P.identB])
    P.op("pool", lambda e: e.affine_select(out=P.ident[:], in_=ones[:], pattern=[[-1, 128]], compare_op=ALU.is_equal,
                                           fill=0.0, base=0, channel_multiplier=1), [P.identB], [P.identB])
    P.op("pool", lambda e: e.tensor_copy(out=P.identb[:], in_=P.ident[:]), [P.identB], [P.identB])


def rms_mod(P, x, xB, A, B, vB, hf, hfB, junk, junkB, st, stB, D, dve="dve"):
    P.op("act", lambda e: e.activation(out=junk, in_=x, func=AF.Square, accum_out=st[:, 0:1]), [xB], [junkB, stB])
    P.op(dve, lambda e: e.tensor_scalar(out=st[:, 1:2], in0=st[:, 0:1], scalar1=1.0 / D, scalar2=EPS, op0=ALU.mult, op1=ALU.add),
         [stB], [stB])
    P.op("act", lambda e: e.activation(out=st[:, 3:4], in_=st[:, 1:2], func=AF.Sqrt), [stB], [stB])
    P.op(dve, lambda e: e.reciprocal(out=st[:, 2:3], in_=st[:, 3:4]), [stB], [stB])
    P.op(dve, lambda e: e.scalar_tensor_tensor(out=hf, in0=x, scalar=st[:, 2:3], in1=A, op0=ALU.mult, op1=ALU.mult),
         [xB, stB, vB], [hfB])
    if B is not None:
        P.op(dve, lambda e: e.tensor_tensor(out=hf, in0=hf, in1=B, op=ALU.add), [hfB, vB], [hfB])


def build_mid(T, D, has_prev, has_mix, two, final, segs, NG=8, NEX=64):
    P = Prog()
    consts(P)
    KC = D // 128
    NT = T // 128
    NR = NG + NEX
    xp = P.dram_in("xp", [T, D])
    if has_prev:
        yp = P.dram_in("yp", [T, D])
    if has_mix:
        ya = P.dram_in("ya", [T, D])
        if two:
            yb = P.dram_in("yb", [T, D])
    vec = P.dram_in("vec", [len(segs), 5, 128, D])
    if final:
        out = P.dram_out("out", [T, D])
    else:
        x1o = P.dram_out("x1", [T, D])
        hTo = P.dram_out("hT", [KC, 128, T], BF16)
        gto = P.dram_out("gates", [T, NEX])
        gso = P.dram_out("gsel", [T, NG])
        wr = P.dram_in("wr", [D, NR])
        br = P.dram_in("br", [128, NR])
        wrs = P.sbuf("wrs", [128, KC, NR], F32); wrB = P.buf("wr")
        brs = P.sbuf("brs", [128, NR], F32)
        P.dma("sp", wrs[:], wr.rearrange("(k p) n -> p k n", p=128), [], [wrB], wrB)
        P.dma("sp", brs[:], br, [], [wrB], wrB)

    vs = P.sbuf("vs", [128, 5, D], F32); vB = P.buf("vs")
    xt = P.sbuf("xt", [128, D], F32); xB = P.buf("xt")
    t1 = P.sbuf("t1", [128, D], F32); t1B = P.buf("t1")
    t2 = P.sbuf("t2", [128, D], F32); t2B = P.buf("t2")
    t3 = P.sbuf("t3", [128, D], F32); t3B = P.buf("t3")
    hf = P.sbuf("hf", [128, D], F32); hfB = P.buf("hf")
    st = P.sbuf("st", [128, 4], F32); stB = P.buf("st")
    if not final:
        h32 = P.sbuf("h32", [128, KC, 128], F32); h32B = P.buf("h32")
        h16 = P.sbuf("h16", [128, KC, 128], BF16); h16B = P.buf("h16")
        ptr = [P.psum("ptr%d" % i, [128, 512], F32) for i in range(2)]
        ptrB = [P.buf() for _ in range(2)]
        plg = P.psum("plg", [128, NR], F32); plgB = P.buf()
        sm = P.sbuf("sm", [128, 256], F32); smB = P.buf("sm")
        g3 = P.sbuf("g3", [128, NEX], F32); g3B = P.buf("g3")
    tri = 0
    for si, (ts, te) in enumerate(segs):
        P.dma("sp", vs[:], vec[si].rearrange("v p d -> p v d"), [], [vB], vB)
        if not final:
            P.op("dve", lambda e: e.scalar_tensor_tensor(out=vs[:, 2, :], in0=vs[:, 3, :], scalar=1.0, in1=vs[:, 2, :],
                                                          op0=ALU.add, op1=ALU.mult), [vB], [vB])
        for ti in range(ts, te):
            r0 = ti * 128
            P.dma("sp", xt[:], xp[r0:r0 + 128, :], [], [xB], xB)
            if has_prev:
                P.dma("sp", t1[:], yp[r0:r0 + 128, :], [], [t1B], t1B)
                P.op("pool", lambda e: e.tensor_tensor(out=t1[:], in0=t1[:], in1=vs[:, 0, :], op=ALU.mult), [t1B, vB], [t1B])
                P.op("pool", lambda e: e.tensor_tensor(out=xt[:], in0=xt[:], in1=t1[:], op=ALU.add), [t1B, xB], [xB])
            if has_mix:
                P.dma("sp", t2[:], ya[r0:r0 + 128, :], [], [t2B], t2B)
                if two:
                    P.dma("sp", t3[:], yb[r0:r0 + 128, :], [], [t3B], t3B)
                    P.op("pool", lambda e: e.tensor_tensor(out=t2[:], in0=t2[:], in1=t3[:], op=ALU.add), [t2B, t3B], [t2B])
                P.op("pool", lambda e: e.tensor_tensor(out=t2[:], in0=t2[:], in1=vs[:, 1, :], op=ALU.mult), [t2B, vB], [t2B])
                P.op("pool", lambda e: e.tensor_tensor(out=xt[:], in0=xt[:], in1=t2[:], op=ALU.add), [t2B, xB], [xB])
            if not final:
                P.dma("sp", x1o[r0:r0 + 128, :], xt[:], [xB], [], xB)
            rms_mod(P, xt[:], xB, vs[:, 2, :], None if final else vs[:, 4, :], vB, hf[:], hfB, t1[:], t1B, st, stB, D)
            if final:
                P.dma("sp", out[r0:r0 + 128, :], hf[:], [hfB], [], hfB)
                continue
            for g in range(KC // 4):
                pt = tri % 2; tri += 1
                for q in range(4):
                    kc = g * 4 + q
                    P.tr(ptr[pt][:, q * 128:(q + 1) * 128], hf[:, kc * 128:(kc + 1) * 128], P.ident[:], [hfB, P.identB], [ptrB[pt]])
                dst = h32[:, g * 4:(g + 1) * 4, :]
                P.op("act", (lambda dst=dst, pt=pt: (lambda e: e.activation(out=dst, in_=ptr[pt][:].rearrange("p (q t) -> p q t", q=4), func=AF.Copy)))(),
                     [ptrB[pt]], [h32B])
            P.op("pool", lambda e: e.tensor_copy(out=h16[:], in_=h32[:]), [h32B], [h16B])
            P.dma("sp", hTo[:, :, r0:r0 + 128].rearrange("k p t -> p k t"), h16[:], [h16B], [], h16B)
            for kc in range(KC):
                P.mm(plg[:], h32[:, kc, :], wrs[:, kc, :], kc == 0, kc == KC - 1, [h32B, wrB], [plgB])
            lg = sm[:, 0:NR]
            P.op("dve", lambda e: e.tensor_tensor(out=lg, in0=plg[:], in1=brs[:], op=ALU.add), [plgB, wrB], [smB])
            gmax, ngmax, sume, pg = sm[:, 80:81], sm[:, 81:82], sm[:, 82:83], sm[:, 83:84]
            ohg = sm[:, 88:96]
            eg = sm[:, 96:104]
            P.op("dve", lambda e: e.reduce_max(out=gmax, in_=sm[:, 0:NG], axis=AX.X), [smB], [smB])
            P.op("dve", lambda e: e.tensor_scalar(out=ohg, in0=sm[:, 0:NG], scalar1=gmax, scalar2=None, op0=ALU.is_equal), [smB], [smB])
            P.op("dve", lambda e: e.tensor_scalar(out=ngmax, in0=gmax, scalar1=-1.0, scalar2=None, op0=ALU.mult), [smB], [smB])
            P.op("act", lambda e: e.activation(out=eg, in_=sm[:, 0:NG], func=AF.Exp, bias=ngmax, accum_out=sume), [smB], [smB])
            P.op("dve", lambda e: e.reciprocal(out=pg, in_=sume), [smB], [smB])
            tmp3 = sm[:, 128:192]
            P.op("dve", lambda e: e.tensor_tensor(out=tmp3.rearrange("p (g j) -> p g j", g=NG),
                                                  in0=sm[:, NG:NR].rearrange("p (g j) -> p g j", g=NG),
                                                  in1=ohg.unsqueeze(2).to_broadcast([128, NG, 8]), op=ALU.mult), [smB], [smB])
            sel = sm[:, 104:112]
            P.op("dve", lambda e: e.tensor_reduce(out=sel, in_=tmp3.rearrange("p (g j) -> p j g", g=NG), axis=AX.X, op=ALU.add), [smB], [smB])
            top8 = sm[:, 112:120]
            P.op("dve", lambda e: e.max(out=top8, in_=sel), [smB], [smB])
            nm1 = sm[:, 84:85]
            P.op("dve", lambda e: e.tensor_scalar(out=nm1, in0=top8[:, 0:1], scalar1=-1.0, scalar2=None, op0=ALU.mult), [smB], [smB])
            ex = sm[:, 120:128]
            P.op("act", lambda e: e.activation(out=ex, in_=sel, func=AF.Exp, bias=nm1), [smB], [smB])
            msk = sm[:, 192:200]
            P.op("dve", lambda e: e.tensor_scalar(out=msk, in0=sel, scalar1=top8[:, 1:2], scalar2=None, op0=ALU.is_ge), [smB], [smB])
            num = sm[:, 200:208]
            P.op("dve", lambda e: e.tensor_tensor(out=num, in0=ex, in1=msk, op=ALU.mult), [smB], [smB])
            den, rden = sm[:, 85:86], sm[:, 86:87]
            P.op("dve", lambda e: e.reduce_sum(out=den, in_=num, axis=AX.X), [smB], [smB])
            P.op("dve", lambda e: e.reciprocal(out=rden, in_=den), [smB], [smB])
            w8 = sm[:, 208:216]
            P.op("dve", lambda e: e.tensor_scalar(out=w8, in0=num, scalar1=rden, scalar2=pg, op0=ALU.mult, op1=ALU.mult), [smB], [smB])
            P.op("dve", lambda e: e.tensor_tensor(out=g3[:].rearrange("p (g j) -> p g j", g=NG),
                                                  in0=ohg.unsqueeze(2).to_broadcast([128, NG, 8]),
                                                  in1=w8.unsqueeze(1).to_broadcast([128, NG, 8]), op=ALU.mult), [smB], [g3B])
            P.dma("sp", gto[r0:r0 + 128, :], g3[:], [g3B], [], g3B)
            P.dma("sp", gso[r0:r0 + 128, :], ohg, [smB], [], smB)
    return P


def ref_mid(xp, yp, ya, yb, vec, segs, wr, br, final):
    T, D = xp.shape
    x = xp.copy()
    h = np.zeros_like(x)
    for si, (ts, te) in enumerate(segs):
        sl = slice(ts * 128, te * 128)
        v = vec[si][:, 0, :]
        if yp is not None:
            x[sl] = x[sl] + v[0] * yp[sl]
        if ya is not None:
            yy = ya[sl] + (yb[sl] if yb is not None else 0)
            x[sl] = x[sl] + v[1] * yy
        n = x[sl] / np.sqrt((x[sl] ** 2).mean(-1, keepdims=True) + EPS)
        if final:
            h[sl] = n * v[2]
        else:
            h[sl] = n * v[2] * (1 + v[3]) + v[4]
    if final:
        return x, h, None
    lg = h @ wr + br[0]
    g, e = lg[:, :8], lg[:, 8:].reshape(T, 8, 8)
    pg = np.exp(g - g.max(-1, keepdims=True)); pg /= pg.sum(-1, keepdims=True)
    gi = g.argmax(-1)
    sel = e[np.arange(T), gi]
    pe = np.exp(sel - sel.max(-1, keepdims=True)); pe /= pe.sum(-1, keepdims=True)
    order = np.argsort(-pe, axis=-1)[:, :2]
    gates = np.zeros((T, 8, 8), np.float32)
    tp = np.take_along_axis(pe, order, -1)
    w = pg[np.arange(T), gi][:, None] * tp / tp.sum(-1, keepdims=True)
    for k in range(2):
        gates[np.arange(T), gi, order[:, k]] = w[:, k]
    return x, h, gates.reshape(T, 64)


def build_moe(T, D, DE, NE, TB=1024):
    P = Prog()
    KC, FC, NS = D // 128, DE // 128, D // 512
    NB = T // TB
    SB = TB // 512
    JT = TB // 128
    hT = P.dram_in("hT", [KC, 128, T], BF16)
    gates = P.dram_in("gates", [T, NE], F32)
    wgu = P.dram_in("wgu", [NE, D, 2 * DE], F32)
    wdn = P.dram_in("wdn", [NE, DE, D], F32)
    y = P.dram_out("y", [T, D], F32)

    hb = P.sbuf("hb", [128, KC, TB], BF16); hbB = P.buf("hb")
    gt = P.sbuf("gt", [128, JT, NE], F32); gtB = P.buf("gt")
    wg = [P.sbuf("wg%d" % i, [128, KC, 2 * DE], BF16) for i in range(2)]
    wd = [P.sbuf("wd%d" % i, [128, FC, D], BF16) for i in range(2)]
    wB = [P.buf("w%d" % i) for i in range(2)]
    sg = [P.sbuf("sg%d" % i, [128, 512], F32) for i in range(2)]
    sgB = [P.buf() for _ in range(2)]
    aT = [P.sbuf("aT%d" % i, [128, FC, 512], BF16) for i in range(2)]
    aTB = [P.buf() for _ in range(2)]
    yacc = P.sbuf("yacc", [128, JT, D], F32)
    yB = [P.buf("y%d" % j) for j in range(JT)]
    pg = [P.psum("pg%d" % i, [128, 512], F32) for i in range(2)]
    pu = [P.psum("pu%d" % i, [128, 512], F32) for i in range(2)]
    pgB = [P.buf() for _ in range(2)]
    puB = [P.buf() for _ in range(2)]
    py = [P.psum("py%d" % i, [128, 512], F32) for i in range(2)]
    pyB = [P.buf() for _ in range(2)]

    hTv = hT.rearrange("k p t -> p k t")
    ci = 0
    ai = 0
    yi = 0
    wi = 0
    for b in range(NB):
        t0 = b * TB
        P.dma("sp", hb[:], hTv[:, :, t0:t0 + TB], [], [hbB], hbB)
        P.dma("sp", gt[:], gates[t0:t0 + TB, :].rearrange("(j p) e -> p j e", p=128), [], [gtB], gtB)
        for e in range(NE):
            ws = wi % 2; wi += 1
            P.dma("pool", wg[ws][:], wgu[e].rearrange("(k p) n -> p k n", p=128), [], [wB[ws]], wB[ws])
            P.dma("pool", wd[ws][:], wdn[e].rearrange("(k p) n -> p k n", p=128), [], [wB[ws]], wB[ws])
            for sb in range(SB):
                a = ai % 2; ai += 1
                for fc in range(FC):
                    c = ci % 2; ci += 1
                    for kc in range(KC):
                        P.mm(pg[c][:], wg[ws][:, kc, fc * 128:(fc + 1) * 128], hb[:, kc, sb * 512:(sb + 1) * 512],
                             kc == 0, kc == KC - 1, [wB[ws], hbB], [pgB[c]])
                    for kc in range(KC):
                        P.mm(pu[c][:], wg[ws][:, kc, DE + fc * 128:DE + (fc + 1) * 128], hb[:, kc, sb * 512:(sb + 1) * 512],
                             kc == 0, kc == KC - 1, [wB[ws], hbB], [puB[c]])
                    P.act(sg[c][:], pg[c][:], AF.Silu, [pgB[c]], [sgB[c]])
                    P.op("dve", (lambda c=c, a=a, fc=fc: (lambda e_: e_.tensor_tensor(out=aT[a][:, fc, :], in0=pu[c][:], in1=sg[c][:], op=ALU.mult)))(),
                         [puB[c], sgB[c]], [aTB[a]])
                for tt in range(4):
                    j = sb * 4 + tt
                    for ns in range(NS):
                        yy = yi % 2; yi += 1
                        for fc in range(FC):
                            P.mm(py[yy][:], aT[a][:, fc, tt * 128:(tt + 1) * 128], wd[ws][:, fc, ns * 512:(ns + 1) * 512],
                                 fc == 0, fc == FC - 1, [aTB[a], wB[ws]], [pyB[yy]])
                        ysl = yacc[:, j, ns * 512:(ns + 1) * 512]
                        gsc = gt[:, j, e:e + 1]
                        if e == 0:
                            P.op("dve", (lambda ysl=ysl, yy=yy, gsc=gsc: (lambda e_: e_.tensor_scalar(out=ysl, in0=py[yy][:], scalar1=gsc, scalar2=None, op0=ALU.mult)))(),
                                 [pyB[yy], gtB], [yB[j]])
                        else:
                            P.op("dve", (lambda ysl=ysl, yy=yy, gsc=gsc: (lambda e_: e_.scalar_tensor_tensor(out=ysl, in0=py[yy][:], scalar=gsc, in1=ysl, op0=ALU.mult, op1=ALU.add)))(),
                                 [pyB[yy], gtB, yB[j]], [yB[j]])
        P.dma("sp", y[t0:t0 + TB, :].rearrange("(j p) d -> p j d", p=128), yacc[:], yB, [], yB[0])
    return P


def build_masks(P):
    names = ["LS", "UI", "US", "LI", "BLK", "H0", "H1"]
    m = {n: P.sbuf("m_" + n, [128, 128], F32) for n in names}
    B = P.buf("masks")
    ones = P.ones
    pool = "pool"
    P.op(pool, lambda e: e.memset(m["BLK"][:], 0.0), [], [B])
    P.op(pool, lambda e: e.memset(m["BLK"][0:64, 0:64], 1.0), [B], [B])
    P.op(pool, lambda e: e.memset(m["BLK"][64:128, 64:128], 1.0), [B], [B])
    P.op(pool, lambda e: e.memset(m["H0"][:], 0.0), [B], [B])
    P.op(pool, lambda e: e.memset(m["H0"][0:64, :], 1.0), [B], [B])
    P.op(pool, lambda e: e.memset(m["H1"][:], 0.0), [B], [B])
    P.op(pool, lambda e: e.memset(m["H1"][64:128, :], 1.0), [B], [B])

    def sel(name, op):
        P.op(pool, lambda e: e.affine_select(out=m[name][:], in_=m["BLK"][:], pattern=[[-1, 128]], compare_op=op,
                                             fill=0.0, base=0, channel_multiplier=1), [B, P.identB], [B])
    sel("LS", ALU.is_gt)
    sel("LI", ALU.is_ge)
    P.op(pool, lambda e: e.tensor_tensor(out=m["UI"][:], in0=m["BLK"][:], in1=m["LS"][:], op=ALU.subtract), [B], [B])
    P.op(pool, lambda e: e.tensor_tensor(out=m["US"][:], in0=m["BLK"][:], in1=m["LI"][:], op=ALU.subtract), [B], [B])
    m["TRIF"] = m["UI"]
    m["TRIB"] = m["LI"]
    return m, B


def build_dn(D, TT, LC, NG, has_prev, DOUT, stage=99, outproj=True):
    P = Prog()
    consts(P)
    M, MB = build_masks(P)
    KC = D // 128
    NTL = TT // 128
    NCOL = 776
    NVH = 2 * NG
    NC4 = NTL * 4
    xp = P.dram_in("xp", [TT, D])
    if has_prev:
        yp = P.dram_in("yp", [TT, D])
    vec = P.dram_in("vec", [2, 5, 128, D])
    win = P.dram_in("win", [NG, D, NCOL])
    cw = P.dram_in("cw", [NG, 128, 4, 5])
    hp = P.dram_in("hp", [NG, 128, 8])
    og = P.dram_in("og", [128, 128])
    if outproj:
        wout = P.dram_in("wout", [NVH * 128, DOUT])
        y = P.dram_out("y", [TT, DOUT])
        ogs = P.dram("ogs", [TT, NVH * 128], BF16)
    else:
        ogs = P.dram_out("og_out", [TT, NVH * 128], BF16)
    hTs = P.dram("hTs", [KC, 128, TT], BF16)
    hTsB = P.buf("hTs")
    ogsB = P.buf("ogs")

    BIGN = max(4 * TT, 3 * D + 3 * D + D, (NVH * DOUT + 1) // 2 + NVH * 128)
    big = P.sbuf("big", [128, BIGN], F32)
    qT, kT, pre, vtmp = (big[:, i * TT:(i + 1) * TT] for i in range(4))
    qTB, kTB, preB, vtmpB = P.buf("qT"), P.buf("kT"), P.buf("pre"), P.buf("vtmp")
    bigBs = [qTB, kTB, preB, vtmpB]

    vs = big[:, 0:3 * D].rearrange("p (v d) -> p v d", v=3)
    xt = big[:, 3 * D:4 * D]
    t1 = big[:, 4 * D:5 * D]
    hf = big[:, 5 * D:6 * D]
    hb16_t = P.sbuf("hb16", [128, D], BF16)
    h16_t = P.sbuf("h16", [128, KC, 128], BF16)
    hb16 = hb16_t[:]
    h16 = h16_t[:]
    vB, xB, t1B, hfB, hb16B, h16B = (P.buf(n) for n in ("vs", "xt", "t1", "hf", "hb16", "h16"))
    st = P.sbuf("st", [128, 4], F32); stB = P.buf("st")
    pbf = P.psum("pbf", [128, 512], BF16); pbfB = P.buf()
    for si, (ts_, te_) in enumerate([(0, LC // 128), (LC // 128, NTL)]):
        P.dma("sp", vs[:, 0, :], vec[si, 0], [], [vB], vB)
        P.dma("sp", vs[:, 1, :], vec[si, 2], [], [vB], vB)
        P.dma("sp", vs[:, 2, :], vec[si, 4], [], [vB], vB)
        P.dma("sp", t1, vec[si, 3], [], [t1B], t1B)
        stt(P, "dve", vs[:, 1, :], t1, 1.0, vs[:, 1, :], ALU.add, ALU.mult, [vB, t1B], [vB])
        for ti in range(ts_, te_):
            r0 = ti * 128
            P.dma("sp", xt, xp[r0:r0 + 128, :], [], [xB], xB)
            if has_prev:
                P.dma("sp", t1, yp[r0:r0 + 128, :], [], [t1B], t1B)
                tt(P, "pool", t1, t1, vs[:, 0, :], ALU.mult, [t1B, vB], [t1B])
                tt(P, "pool", xt, xt, t1, ALU.add, [t1B, xB], [xB])
            rms_mod(P, xt, xB, vs[:, 1, :], vs[:, 2, :], vB, hf, hfB, t1, t1B, st, stB, D)
            cp(P, "pool", hb16, hf, [hfB], [hb16B])
            for g4 in range(0, KC, 4):
                n4 = min(4, KC - g4)
                for q in range(n4):
                    kc = g4 + q
                    P.tr(pbf[:, q * 128:(q + 1) * 128], hb16[:, kc * 128:(kc + 1) * 128], P.identb[:], [hb16B, P.identB], [pbfB])
                cp(P, "act", h16[:, g4:g4 + n4, :], pbf[:, 0:n4 * 128].rearrange("p (q t) -> p q t", q=n4), [pbfB], [h16B])
            P.dma("sp", hTs[:, :, r0:r0 + 128].rearrange("k p t -> p k t"), h16, [h16B], [hTsB], h16B)

    if stage <= 1:
        return P
    wg = P.sbuf("wg", [128, KC, NCOL], BF16); wgB = P.buf("wg")
    cws = P.sbuf("cws", [128, 4, 5], F32)
    hps = P.sbuf("hps", [128, 8], F32)
    ogt = P.sbuf("ogt", [128, 128], F32); ogB = P.buf("og")
    P.dma("sp", ogt[:], og, [], [ogB], ogB)
    hb = P.sbuf("hb", [128, KC, 256], BF16); hbB = P.buf("hb")
    k_tok = P.sbuf("k_tok", [128, NTL, 128], F32); ktB = P.buf("k_tok")
    v_tok = P.sbuf("v_tok", [128, NTL, 128], BF16); vtB = P.buf("v_tok")
    zs = P.sbuf("zs", [128, NTL, 128], BF16); zsB = P.buf("zs")
    o_acc = P.sbuf("o_acc", [128, NTL, 128], F32)
    oB = [P.buf("oacc%d" % i) for i in range(NTL)]
    ofin = P.sbuf("ofin", [128, NTL, 128], BF16); ofB = P.buf("ofin")
    sm = {n: P.sbuf("sm_" + n, [128, NTL, 2], F32) for n in ("A", "Bq", "G", "BETA", "NBETA", "GC", "EG", "BEXP", "GLT", "EDK", "EGL0", "EGL1", "T")}
    smB = P.buf("sm")
    AB = P.sbuf("AB", [128, NTL, 4], F32)
    pp = [P.psum("pp%d" % i, [128, 512], F32) for i in range(2)]
    ppB = [P.buf() for _ in range(2)]
    tmpA = P.sbuf("tmpA", [128, 256], F32); tmpAB = P.buf()
    tmpC = P.sbuf("tmpC", [128, 256], F32); tmpCB = P.buf()
    psb = [P.psum("psb%d" % i, [128, 512], F32) for i in range(5)]
    psB = [P.buf() for _ in range(5)]
    psi = [0]

    def ps_next(kind="d"):
        i = psi[0] % 5
        psi[0] += 1
        return psb[i][:, 0:128], psB[i]
    NTMP = 28
    tmps = P.sbuf("tmps", [128, NTMP, 128], F32)
    tmpB = [P.buf() for _ in range(NTMP)]
    tpi = [0]

    def tmp_next():
        i = tpi[0] % NTMP
        tpi[0] += 1
        return tmps[:, i, :], tmpB[i]
    S = [P.sbuf("S%d" % i, [128, 128], F32) for i in range(2)]
    SB = [P.buf("S%d" % i) for i in range(2)]
    ppi = [0]
    blocks = [(b0, min(256, TT - b0)) for b0 in range(0, TT, 256)]

    def load_hb(b0, bn):
        P.dma("sp", hb[:, :, 0:bn], hTs[:, :, b0:b0 + bn].rearrange("k p t -> p k t"), [hTsB], [hbB], hbB)

    def proj_fm(col0, dst, dstB):
        for (b0, bn) in blocks:
            load_hb(b0, bn)
            pi = ppi[0] % 2; ppi[0] += 1
            for kc in range(KC):
                P.mm(pp[pi][:, 0:bn], wg[:, kc, col0:col0 + 128], hb[:, kc, 0:bn], kc == 0, kc == KC - 1, [wgB, hbB], [ppB[pi]])
            cp(P, "act", dst[:, b0:b0 + bn], pp[pi][:, 0:bn], [ppB[pi]], [dstB])

    def conv_silu(src, srcB, dst, dstB, j):
        for (lo, hi) in ((0, LC), (LC, TT)):
            ts(P, "dve", dst[:, lo:hi], src[:, lo:hi], cws[:, j, 2:3], None, ALU.mult, None, [srcB, wgB], [dstB])
            for tap in (0, 1, 3, 4):
                off = tap - 2
                a, b = lo + max(0, -off), hi - max(0, off)
                stt(P, "dve", dst[:, a:b], src[:, a + off:b + off], cws[:, j, tap:tap + 1], dst[:, a:b], ALU.mult, ALU.add,
                    [srcB, dstB, wgB], [dstB])
        actf(P, dst, dst, AF.Silu, [dstB], [dstB])

    def l2norm(dst, dstB, scale):
        for (b0, bn) in blocks:
            actf(P, tmpA[:, 0:bn], dst[:, b0:b0 + bn], AF.Square, [dstB], [tmpAB])
            pi = ppi[0] % 2; ppi[0] += 1
            P.mm(pp[pi][:, 0:bn], P.ones[:], tmpA[:, 0:bn], True, True, [tmpAB, P.identB], [ppB[pi]])
            ts(P, "dve", tmpC[:, 0:bn], pp[pi][:, 0:bn], EPS, None, ALU.add, None, [ppB[pi]], [tmpCB])
            actf(P, tmpC[:, 0:bn], tmpC[:, 0:bn], AF.Sqrt, [tmpCB], [tmpCB])
            P.op("dve", (lambda bn=bn: (lambda e: e.reciprocal(out=tmpC[:, 0:bn], in_=tmpC[:, 0:bn])))(), [tmpCB], [tmpCB])
            stt(P, "dve", dst[:, b0:b0 + bn], dst[:, b0:b0 + bn], scale, tmpC[:, 0:bn], ALU.mult, ALU.mult, [dstB, tmpCB], [dstB])

    def to_tok(src, srcB, dst, dstB):
        for t4 in range(0, NTL, 4):
            n4 = min(4, NTL - t4)
            pi = ppi[0] % 2; ppi[0] += 1
            for q in range(n4):
                P.tr(pp[pi][:, q * 128:(q + 1) * 128], src[:, (t4 + q) * 128:(t4 + q + 1) * 128], P.ident[:], [srcB, P.identB], [ppB[pi]])
            cp(P, "act", dst[:, t4:t4 + n4, :], pp[pi][:, 0:n4 * 128].rearrange("p (q t) -> p q t", q=n4), [ppB[pi]], [dstB])

    orders = [list(range(NTL)), list(range(LC // 128 - 1, -1, -1)) + list(range(NTL - 1, LC // 128 - 1, -1))]

    for g in range(NG):
        P.dma("pool", wg[:], win[g].rearrange("(k p) n -> p k n", p=128), [], [wgB], wgB)
        P.dma("sp", cws[:], cw[g], [], [wgB], wgB)
        P.dma("sp", hps[:], hp[g], [], [wgB], wgB)
        if stage == 20:
            return P
        proj_fm(0, pre, preB)
        if stage == 21:
            return P
        conv_silu(pre, preB, qT, qTB, 0)
        if stage == 22:
            return P
        l2norm(qT, qTB, 128.0 ** -0.5)
        if stage == 23:
            return P
        proj_fm(128, pre, preB)
        conv_silu(pre, preB, kT, kTB, 1)
        l2norm(kT, kTB, 1.0)
        to_tok(kT, kTB, k_tok, ktB)
        if stage <= 2:
            return P
        for vh in range(2):
            cb = 256 + vh * 260
            proj_fm(cb, pre, preB)
            conv_silu(pre, preB, vtmp, vtmpB, 2 + vh)
            to_tok(vtmp, vtmpB, v_tok, vtB)
            if stage == 30:
                return P
            for (b0, bn) in blocks:
                load_hb(b0, bn)
                for tq in range(bn // 128):
                    ti = b0 // 128 + tq
                    pi = ppi[0] % 2; ppi[0] += 1
                    for kc in range(KC):
                        P.mm(pp[pi][:, 0:132], hb[:, kc, tq * 128:(tq + 1) * 128], wg[:, kc, cb + 128:cb + 260], kc == 0, kc == KC - 1,
                             [wgB, hbB], [ppB[pi]])
                    actf(P, zs[:, ti, :], pp[pi][:, 0:128], AF.Silu, [ppB[pi]], [zsB])
                    cp(P, "act", AB[:, ti, :], pp[pi][:, 128:132], [ppB[pi]], [smB])
            if stage == 31:
                return P
            hv = hps[:, vh * 4:vh * 4 + 4]
            tt(P, "dve", sm["T"][:], AB[:, :, 0:2], hv[:, 2:4].unsqueeze(1).to_broadcast([128, NTL, 2]), ALU.add, [smB, wgB], [smB])
            actf(P, sm["T"][:], sm["T"][:], AF.Exp, [smB], [smB])
            ts(P, "dve", sm["T"][:], sm["T"][:], 1.0, None, ALU.add, None, [smB], [smB])
            actf(P, sm["T"][:], sm["T"][:], AF.Ln, [smB], [smB])
            actf(P, sm["EGL0"][:, 0, :], hv[:, 0:2], AF.Exp, [wgB, smB], [smB])
            ts(P, "dve", sm["EGL0"][:, 0, :], sm["EGL0"][:, 0, :], -1.0, None, ALU.mult, None, [smB], [smB])
            tt(P, "dve", sm["G"][:], sm["T"][:], sm["EGL0"][:, 0, :].unsqueeze(1).to_broadcast([128, NTL, 2]), ALU.mult, [smB], [smB])
            actf(P, sm["BETA"][:], AB[:, :, 2:4], AF.Sigmoid, [smB], [smB])
            ts(P, "dve", sm["NBETA"][:], sm["BETA"][:], -1.0, None, ALU.mult, None, [smB], [smB])
            if stage == 32:
                return P
            Gf = sm["G"][:].rearrange("p t c -> p (t c)")
            for d, mk in ((0, "TRIF"), (1, "TRIB")):
                pi = ppi[0] % 2; ppi[0] += 1
                P.mm(pp[pi][:, 0:2 * NTL], M[mk][:], Gf, True, True, [smB, MB], [ppB[pi]])
                cp(P, "dve", sm["GC"][:, :, d:d + 1], pp[pi][:, 0:2 * NTL].rearrange("p (t c) -> p t c", c=2)[:, :, d:d + 1], [ppB[pi]], [smB])
            pi = ppi[0] % 2; ppi[0] += 1
            P.mm(pp[pi][:, 0:2 * NTL], M["BLK"][:], Gf, True, True, [smB, MB], [ppB[pi]])
            cp(P, "dve", sm["GLT"][:].rearrange("p t c -> p (t c)"), pp[pi][:, 0:2 * NTL], [ppB[pi]], [smB])
            for h, nm in ((0, "EGL0"), (1, "EGL1")):
                pi = ppi[0] % 2; ppi[0] += 1
                P.mm(pp[pi][:, 0:2 * NTL], M["H%d" % h][:], Gf, True, True, [smB, MB], [ppB[pi]])
                actf(P, sm[nm][:].rearrange("p t c -> p (t c)"), pp[pi][:, 0:2 * NTL], AF.Exp, [ppB[pi]], [smB])
            actf(P, sm["EG"][:], sm["GC"][:], AF.Exp, [smB], [smB])
            tt(P, "dve", sm["BEXP"][:], sm["BETA"][:], sm["EG"][:], ALU.mult, [smB], [smB])
            tt(P, "dve", sm["EDK"][:], sm["GLT"][:], sm["GC"][:], ALU.subtract, [smB], [smB])
            actf(P, sm["EDK"][:], sm["EDK"][:], AF.Exp, [smB], [smB])
            if stage <= 3:
                return P
            for d in range(2):
                P.op("pool", (lambda d=d: (lambda e: e.memset(S[d][:], 0.0)))(), [], [SB[d]])
            written = [False] * NTL
            for step in range(NTL):
                for d in range(2):
                    ti = orders[d][step]
                    c0 = ti * 128
                    col = lambda nm: sm[nm][:, ti, d:d + 1]
                    mS, mT = (("LS", "UI") if d == 0 else ("US", "LI"))
                    dg, dgB = tmp_next()
                    ts(P, "dve", dg, P.ident[:], col("GC"), None, ALU.mult, None, [smB, P.identB], [dgB])
                    pgr, pgrB = ps_next()
                    P.mm(pgr, P.ones[:], dg, True, True, [dgB, P.identB], [pgrB])
                    E, EB = tmp_next()
                    ts(P, "dve", E, pgr, col("GC"), None, ALU.subtract, None, [pgrB, smB], [EB])
                    stt(P, "dve", E, E, -1.0, E, ALU.mult, ALU.max, [EB], [EB])
                    actf(P, E, E, AF.Exp, [EB], [EB], scale=-1.0)
                    Dm, DmB = tmp_next()
                    tt(P, "pool", Dm, E, M[mS][:], ALU.mult, [EB, MB], [DmB])
                    DmT, DmTB = tmp_next()
                    tt(P, "pool", DmT, E, M[mT][:], ALU.mult, [EB, MB], [DmTB])
                    egr, egrB = tmp_next()
                    actf(P, egr, pgr, AF.Exp, [pgrB], [egrB])
                    if stage == 40:
                        return P
                    pkk, pkkB = ps_next()
                    k2, k2B = tmp_next()
                    cp(P, "pool", k2, kT[:, c0:c0 + 128], [kTB], [k2B])
                    P.mm(pkk, kT[:, c0:c0 + 128], k2[:], True, True, [kTB, k2B], [pkkB])
                    if stage == 401:
                        return P
                    pkq, pkqB = ps_next()
                    P.mm(pkq, kT[:, c0:c0 + 128], qT[:, c0:c0 + 128], True, True, [kTB, qTB], [pkqB])
                    if stage == 402:
                        return P
                    N, NB = tmp_next()
                    stt(P, "dve", N, pkk, col("NBETA"), Dm, ALU.mult, ALU.mult, [pkkB, smB, DmB], [NB])
                    if stage == 403:
                        return P
                    qkT, qkTB = tmp_next()
                    tt(P, "dve", qkT, pkq, DmT, ALU.mult, [pkqB, DmTB], [qkTB])
                    if stage == 41:
                        return P
                    pnt, pntB = ps_next("a")
                    P.tr(pnt, N, P.ident[:], [NB, P.identB], [pntB])
                    NT, NTB = tmp_next()
                    cp(P, "act", NT, pnt, [pntB], [NTB])
                    XT, XTB = tmp_next()
                    tt(P, "pool", XT, NT, P.ident[:], ALU.add, [NTB, P.identB], [XTB])
                    if stage == 42:
                        return P
                    for lvl in range(5):
                        pn2, pn2B = ps_next("a")
                        P.mm(pn2, NT, N, True, True, [NTB, NB], [pn2B])
                        N2, N2B = tmp_next()
                        cp(P, "act", N2, pn2, [pn2B], [N2B])
                        if lvl < 4:
                            pn2t, pn2tB = ps_next("a")
                            P.mm(pn2t, N, NT, True, True, [NTB, NB], [pn2tB])
                            N2T, N2TB = tmp_next()
                            cp(P, "act", N2T, pn2t, [pn2tB], [N2TB])
                        px, pxB = ps_next()
                        P.mm(px, N2, XT, True, True, [N2B, XTB], [pxB])
                        tt(P, "dve", XT, XT, px, ALU.add, [XTB, pxB], [XTB])
                        N, NB = N2, N2B
                        if lvl < 4:
                            NT, NTB = N2T, N2TB
                    if stage == 421:
                        return P
                    vb, vbB = tmp_next()
                    ts(P, "dve", vb, v_tok[:, ti, :], col("BETA"), None, ALU.mult, None, [vtB, smB], [vbB])
                    kbg, kbgB = tmp_next()
                    ts(P, "dve", kbg, k_tok[:, ti, :], col("BEXP"), None, ALU.mult, None, [ktB, smB], [kbgB])
                    pu, puB = ps_next("a")
                    P.mm(pu, XT, vb, True, True, [XTB, vbB], [puB])
                    u, uB = tmp_next()
                    cp(P, "act", u, pu, [puB], [uB])
                    if stage == 422:
                        return P
                    pw, pwB = ps_next("a")
                    P.mm(pw, kbg, XT, True, True, [kbgB, XTB], [pwB])
                    wT, wTB = tmp_next()
                    cp(P, "act", wT, pw, [pwB], [wTB])
                    if stage == 43:
                        return P
                    qdT, qdTB = tmp_next()
                    tt(P, "dve", qdT, qT[:, c0:c0 + 128], egr, ALU.mult, [qTB, egrB], [qdTB])
                    kdec, kdecB = tmp_next()
                    ts(P, "dve", kdec, k_tok[:, ti, :], col("EDK"), None, ALU.mult, None, [ktB, smB], [kdecB])
                    if stage == 44:
                        return P
                    vnew, vnewB = tmp_next()
                    for h in ((0, 1) if d == 0 else (1, 0)):
                        rs = slice(h * 64, h * 64 + 64)
                        ppv, ppvB = ps_next()
                        P.mm(ppv, wT, S[d][:], True, True, [wTB, SB[d]], [ppvB])
                        tt(P, "dve", vnew[rs, :], u[rs, :], ppv[rs, :], ALU.subtract, [uB, ppvB], [vnewB])
                        po, poB = ps_next()
                        P.mm(po, qdT, S[d][:], True, False, [qdTB, SB[d]], [poB])
                        P.mm(po, qkT[rs, :], vnew[rs, :], False, True, [qkTB, vnewB], [poB])
                        if not written[ti]:
                            cp(P, "act", o_acc[rs, ti, :], po[rs, :], [poB], [oB[ti]])
                        else:
                            tt(P, "dve", o_acc[rs, ti, :], o_acc[rs, ti, :], po[rs, :], ALU.add, [poB, oB[ti]], [oB[ti]])
                        pS, pSB = ps_next()
                        P.mm(pS, kdec[rs, :], vnew[rs, :], True, True, [kdecB, vnewB], [pSB])
                        glc = sm["EGL%d" % h][:, ti, d:d + 1]
                        stt(P, "dve", S[d][:], S[d][:], glc, pS, ALU.mult, ALU.add, [SB[d], smB, pSB], [SB[d]])
                    if written[ti] is False:
                        written[ti] = "half"
                    else:
                        written[ti] = True
                for i in range(NTL):
                    if written[i] == "half":
                        written[i] = True
            if stage <= 4:
                return P
            for ti in range(NTL):
                jk, jkB = tmp_next()
                actf(P, jk, o_acc[:, ti, :], AF.Square, [oB[ti]], [jkB, stB], accum_out=st[:, 0:1])
                ts(P, "dve", st[:, 1:2], st[:, 0:1], 1.0 / 128, EPS, ALU.mult, ALU.add, [stB], [stB])
                actf(P, st[:, 3:4], st[:, 1:2], AF.Sqrt, [stB], [stB])
                P.op("dve", lambda e: e.reciprocal(out=st[:, 2:3], in_=st[:, 3:4]), [stB], [stB])
                stt(P, "dve", jk, o_acc[:, ti, :], st[:, 2:3], ogt[:], ALU.mult, ALU.mult, [oB[ti], stB, ogB], [jkB])
                tt(P, "dve", ofin[:, ti, :], jk, zs[:, ti, :], ALU.mult, [jkB, zsB], [ofB])
            hh = g * 2 + vh
            P.dma("sp", ogs[:, hh * 128:(hh + 1) * 128].rearrange("(t p) d -> p t d", p=128), ofin[:], [ofB], [ogsB], ofB)

    if stage <= 5 or not outproj:
        return P
    wo_t = P.sbuf("wo", [128, NVH, DOUT], BF16)
    wo = wo_t[:]
    woB = P.buf("wo")
    P.dma("pool", wo, wout.rearrange("(k p) n -> p k n", p=128), [], [woB], woB)
    ot_t = P.sbuf("ot", [128, NVH * 128], BF16)
    ot = ot_t[:]
    otB = P.buf("ot")
    oT = P.sbuf("oT", [128, NVH, 128], BF16); oTB = P.buf("oT")
    yt = P.sbuf("yt", [128, DOUT], F32); ytB = P.buf("yt")
    for ti in range(NTL):
        r0 = ti * 128
        P.dma("sp", ot, ogs[r0:r0 + 128, :], [ogsB], [otB], otB)
        for g4 in range(0, NVH, 4):
            n4 = min(4, NVH - g4)
            for q in range(n4):
                P.tr(pbf[:, q * 128:(q + 1) * 128], ot[:, (g4 + q) * 128:(g4 + q + 1) * 128], P.identb[:], [otB, P.identB], [pbfB])
            cp(P, "act", oT[:, g4:g4 + n4, :], pbf[:, 0:n4 * 128].rearrange("p (q t) -> p q t", q=n4), [pbfB], [oTB])
        NSW = min(512, DOUT)
        for ns in range(0, DOUT, NSW):
            pi = ppi[0] % 2; ppi[0] += 1
            for k in range(NVH):
                P.mm(pp[pi][:, 0:NSW], oT[:, k, :], wo[:, k, ns:ns + NSW], k == 0, k == NVH - 1, [oTB, woB], [ppB[pi]])
            cp(P, "act", yt[:, ns:ns + NSW], pp[pi][:, 0:NSW], [ppB[pi]], [ytB])
        P.dma("sp", y[r0:r0 + 128, :], yt[:], [ytB], [], ytB)
    return P


HALO = 15
NTAP = 31


def build_conf(D, subs, has_prev):
    P = Prog()
    consts(P)
    KC = D // 128
    CI = D
    NCH = CI // 128
    TSUM = sum(s[0] for s in subs)
    VSUM = sum(s[1] for s in subs)
    TSMAX = max(s[0] for s in subs)
    TVMAX = max(s[1] for s in subs)
    xp = P.dram_in("xp", [TSUM, D])
    if has_prev:
        yp = P.dram_in("yp", [TSUM, D])
    vec = P.dram_in("vec", [2, 5, 128, D])
    mask = P.dram_in("mask", [128, TSUM])
    w1 = P.dram_in("w1", [NCH, D, 256])
    cb = P.dram_in("cb", [128, NCH, 6 + NTAP])
    w2 = P.dram_in("w2", [CI, D])
    b2 = P.dram_in("b2", [128, D])
    y = P.dram_out("y", [VSUM, D])

    vs = P.sbuf("vs", [128, 3, D], F32); vB = P.buf("vs")
    CVW = max(TVMAX, (3 * D + NCH - 1) // NCH)
    cv = P.sbuf("cv", [128, NCH, CVW], F32); cvB = P.buf("cv")
    cvf = cv[:].rearrange("p c t -> p (c t)")
    xt, t1, hf = cvf[:, 0:D], cvf[:, D:2 * D], cvf[:, 2 * D:3 * D]
    xB, t1B, hfB = P.buf("xt"), P.buf("t1"), P.buf("hf")
    hb16 = P.sbuf("hb16", [128, D], BF16); hb16B = P.buf("hb16")
    hT = P.sbuf("hT", [128, KC, TSMAX], BF16); hTB = P.buf("hT")
    uT = P.sbuf("uT", [128, NCH, TSMAX], BF16); uTB = P.buf("uT")
    vT = P.sbuf("vT", [128, NCH, TVMAX], BF16); vTB = P.buf("vT")
    wc = [P.sbuf("wc%d" % i, [128, KC, 256], BF16) for i in range(2)]
    wcB = [P.buf("wc%d" % i) for i in range(2)]
    w2s = [P.sbuf("w2s%d" % i, [128, NCH, 512], BF16) for i in range(2)]
    w2B = [P.buf("w2s%d" % i) for i in range(2)]
    cbs = P.sbuf("cbs", [128, NCH, 6 + NTAP], F32); cbB = P.buf("cb")
    b2s = P.sbuf("b2s", [128, D], F32)
    mk = P.sbuf("mk", [128, TSMAX], F32); mkB = P.buf("mk")
    st = P.sbuf("st", [128, 4], F32); stB = P.buf("st")
    mean = P.sbuf("mean", [128, 512], F32); meanB = P.buf("mean")
    rstd = P.sbuf("rstd", [128, 512], F32); rstdB = P.buf("rstd")
    tA = P.sbuf("tA", [128, 512], F32); tAB = P.buf("tA")
    tC = P.sbuf("tC", [128, 512], F32); tCB = P.buf("tC")
    ys = [P.sbuf("ys%d" % i, [128, 512], F32) for i in range(2)]
    ysB = [P.buf("ys%d" % i) for i in range(2)]
    pbf = P.psum("pbf", [128, 512], BF16); pbfB = P.buf()
    pa = [P.psum("pa%d" % i, [128, 512], F32) for i in range(2)]
    paB = [P.buf() for _ in range(2)]
    pg = [P.psum("pg%d" % i, [128, 512], F32) for i in range(2)]
    pgB = [P.buf() for _ in range(2)]
    p1 = P.psum("p1", [128, 512], F32); p1B = P.buf()
    p2 = P.psum("p2", [128, 512], F32); p2B = P.buf()

    P.dma("sp", cbs[:], cb, [], [cbB], cbB)
    P.dma("sp", b2s[:], b2, [], [cbB], cbB)
    wi = 0
    w2i = 0
    ci = 0
    yi = 0
    tok0 = 0
    out0 = 0
    cur_seg = None
    for (TS, TV, seg) in subs:
        NT = TS // 128
        if seg != cur_seg:
            cur_seg = seg
            P.dma("sp", vs[:, 0, :], vec[seg, 0], [], [vB], vB)
            P.dma("sp", vs[:, 1, :], vec[seg, 2], [], [vB], vB)
            P.dma("sp", vs[:, 2, :], vec[seg, 4], [], [vB], vB)
            P.dma("sp", t1, vec[seg, 3], [], [t1B, cvB], t1B)
            stt(P, "dve", vs[:, 1, :], t1, 1.0, vs[:, 1, :], ALU.add, ALU.mult, [vB, t1B], [vB])
        P.dma("sp", mk[:, 0:TS], mask[:, tok0:tok0 + TS], [], [mkB], mkB)
        for ti in range(NT):
            r0 = tok0 + ti * 128
            P.dma("sp", xt, xp[r0:r0 + 128, :], [], [xB, cvB], xB)
            if has_prev:
                P.dma("sp", t1, yp[r0:r0 + 128, :], [], [t1B], t1B)
                tt(P, "pool", t1, t1, vs[:, 0, :], ALU.mult, [t1B, vB], [t1B])
                tt(P, "pool", xt, xt, t1, ALU.add, [t1B, xB], [xB])
            rms_mod(P, xt, xB, vs[:, 1, :], vs[:, 2, :], vB, hf, hfB, t1, t1B, st, stB, D)
            cp(P, "pool", hb16[:], hf, [hfB], [hb16B])
            for g4 in range(0, KC, 4):
                n4 = min(4, KC - g4)
                for q in range(n4):
                    kc = g4 + q
                    P.tr(pbf[:, q * 128:(q + 1) * 128], hb16[:, kc * 128:(kc + 1) * 128], P.identb[:], [hb16B, P.identB], [pbfB])
                cp(P, "act", hT[:, g4:g4 + n4, ti * 128:(ti + 1) * 128], pbf[:, 0:n4 * 128].rearrange("p (q t) -> p q t", q=n4),
                   [pbfB], [hTB])
        blocks = [(b0, min(512, TS - b0)) for b0 in range(0, TS, 512)]
        for c in range(NCH):
            ws = wi % 2; wi += 1
            P.dma("pool", wc[ws][:], w1[c].rearrange("(k p) n -> p k n", p=128), [], [wcB[ws]], wcB[ws])
            for (b0, bn) in blocks:
                k = ci % 2; ci += 1
                for kc in range(KC):
                    P.mm(pa[k][:, 0:bn], wc[ws][:, kc, 0:128], hT[:, kc, b0:b0 + bn], kc == 0, kc == KC - 1, [wcB[ws], hTB], [paB[k]])
                for kc in range(KC):
                    P.mm(pg[k][:, 0:bn], wc[ws][:, kc, 128:256], hT[:, kc, b0:b0 + bn], kc == 0, kc == KC - 1, [wcB[ws], hTB], [pgB[k]])
                actf(P, tA[:, 0:bn], pg[k][:, 0:bn], AF.Sigmoid, [pgB[k], cbB], [tAB], bias=cbs[:, c, 1:2])
                stt(P, "dve", tA[:, 0:bn], pa[k][:, 0:bn], cbs[:, c, 0:1], tA[:, 0:bn], ALU.add, ALU.mult, [paB[k], tAB, cbB], [tAB])
                tt(P, "dve", uT[:, c, b0:b0 + bn], tA[:, 0:bn], mk[:, b0:b0 + bn], ALU.mult, [tAB, mkB], [uTB])
            ts(P, "dve", cv[:, c, 0:TV], uT[:, c, 0:TV], cbs[:, c, 6:7], cbs[:, c, 2:3], ALU.mult, ALU.add,
               [uTB, cbB, xB, t1B, hfB], [cvB])
            for tap in range(1, NTAP):
                stt(P, "dve", cv[:, c, 0:TV], uT[:, c, tap:tap + TV], cbs[:, c, 6 + tap:7 + tap], cv[:, c, 0:TV], ALU.mult, ALU.add,
                    [uTB, cbB, cvB], [cvB])
        for c in range(NCH):
            P.mm(p1[:, 0:TV], P.ones[:], cv[:, c, 0:TV], c == 0, c == NCH - 1, [cvB, P.identB], [p1B])
        for c in range(NCH):
            actf(P, tC[:, 0:TV], cv[:, c, 0:TV], AF.Square, [cvB], [tCB])
            P.mm(p2[:, 0:TV], P.ones[:], tC[:, 0:TV], c == 0, c == NCH - 1, [tCB, P.identB], [p2B])
        actf(P, mean[:, 0:TV], p1[:, 0:TV], AF.Copy, [p1B], [meanB], scale=1.0 / CI)
        tt(P, "dve", tA[:, 0:TV], mean[:, 0:TV], mean[:, 0:TV], ALU.mult, [meanB], [tAB])
        stt(P, "dve", rstd[:, 0:TV], p2[:, 0:TV], 1.0 / CI, tA[:, 0:TV], ALU.mult, ALU.subtract, [p2B, tAB], [rstdB])
        ts(P, "dve", rstd[:, 0:TV], rstd[:, 0:TV], EPS, None, ALU.add, None, [rstdB], [rstdB])
        actf(P, rstd[:, 0:TV], rstd[:, 0:TV], AF.Sqrt, [rstdB], [rstdB])
        P.op("dve", (lambda TV=TV: (lambda e: e.reciprocal(out=rstd[:, 0:TV], in_=rstd[:, 0:TV])))(), [rstdB], [rstdB])
        for c in range(NCH):
            tt(P, "dve", tA[:, 0:TV], cv[:, c, 0:TV], mean[:, 0:TV], ALU.subtract, [cvB, meanB], [tAB])
            tt(P, "dve", tA[:, 0:TV], tA[:, 0:TV], rstd[:, 0:TV], ALU.mult, [tAB, rstdB], [tAB])
            actf(P, vT[:, c, 0:TV], tA[:, 0:TV], AF.Silu, [tAB, cbB], [vTB], scale=cbs[:, c, 3:4], bias=cbs[:, c, 4:5])
        NSW = min(512, D)
        for ns in range(0, D, NSW):
            w = w2i % 2; w2i += 1
            P.dma("pool", w2s[w][:, :, 0:NSW], w2[:, ns:ns + NSW].rearrange("(k p) n -> p k n", p=128), [], [w2B[w]], w2B[w])
            for tq in range(TV // 128):
                k = ci % 2; ci += 1
                for c in range(NCH):
                    P.mm(pa[k][:, 0:NSW], vT[:, c, tq * 128:(tq + 1) * 128], w2s[w][:, c, 0:NSW], c == 0, c == NCH - 1,
                         [vTB, w2B[w]], [paB[k]])
                yy = yi % 2; yi += 1
                tt(P, "dve", ys[yy][:, 0:NSW], pa[k][:, 0:NSW], b2s[:, ns:ns + NSW], ALU.add, [paB[k], cbB], [ysB[yy]])
                P.dma("sp", y[out0 + tq * 128:out0 + (tq + 1) * 128, ns:ns + NSW], ys[yy][:, 0:NSW], [ysB[yy]], [], ysB[yy])
        tok0 += TS
        out0 += TV
    return P


def arrange_conf_w(cv_w1, cv_b1, cv_dw, cv_dwb, cv_ln_g, cv_ln_b):
    CI = cv_dw.shape[1]
    NCH = CI // 128
    w1 = np.stack([np.concatenate([cv_w1[:, c * 128:(c + 1) * 128], cv_w1[:, CI + c * 128:CI + (c + 1) * 128]], axis=1)
                   for c in range(NCH)])
    cb = np.zeros((128, NCH, 6 + NTAP), np.float32)
    for c in range(NCH):
        sl = slice(c * 128, (c + 1) * 128)
        cb[:, c, 0] = cv_b1[sl]
        cb[:, c, 1] = cv_b1[CI + c * 128:CI + (c + 1) * 128]
        cb[:, c, 2] = cv_dwb[sl]
        cb[:, c, 3] = cv_ln_g[sl]
        cb[:, c, 4] = cv_ln_b[sl]
        cb[:, c, 6:] = cv_dw[:, sl].T
    return np.ascontiguousarray(w1), cb


def build_mod(D, NCOLS, NL, M=5):
    P = Prog()
    KC = D // 128
    ccT = P.dram_in("ccT", [128, KC, M])
    aw = P.dram_in("aw", [NL, D, NCOLS])
    ab = P.dram_in("ab", [NL, M, NCOLS])
    out = P.dram_out("mod", [NL, M, NCOLS])
    cs = P.sbuf("cs", [128, KC, M], F32); csB = P.buf("cs")
    P.dma("sp", cs[:], ccT, [], [csB], csB)
    actf(P, cs[:], cs[:], AF.Silu, [csB], [csB])
    bt = P.sbuf("bt", [M, NL, NCOLS], F32); btB = P.buf("bt")
    P.dma("sp", bt[:], ab.rearrange("l m n -> m l n"), [], [btB], btB)
    wt = [P.sbuf("wt%d" % i, [128, KC, 512], F32) for i in range(2)]
    wtB = [P.buf("wt%d" % i) for i in range(2)]
    ot = [P.sbuf("ot%d" % i, [M, 512], F32) for i in range(2)]
    otB = [P.buf("ot%d" % i) for i in range(2)]
    pm = [P.psum("pm%d" % i, [128, 512], F32) for i in range(2)]
    pmB = [P.buf() for _ in range(2)]
    i = 0
    for l in range(NL):
        for ns in range(0, NCOLS, 512):
            w = i % 2; i += 1
            P.dma("sp", wt[w][:], aw[l][:, ns:ns + 512].rearrange("(k p) n -> p k n", p=128), [], [wtB[w]], wtB[w])
            for kc in range(KC):
                P.mm(pm[w][0:M, :], cs[:, kc, :], wt[w][:, kc, :], kc == 0, kc == KC - 1, [csB, wtB[w]], [pmB[w]])
            tt(P, "dve", ot[w][:], pm[w][0:M, :], bt[:, l, ns:ns + 512], ALU.add, [pmB[w], btB], [otB[w]])
            P.dma("sp", out[l][:, ns:ns + 512], ot[w][:], [otB[w]], [], otB[w])
    return P


def build_lin(T, K, N):
    P = Prog()
    consts(P)
    KC = K // 128
    x = P.dram_in("x", [T, K], BF16)
    w = P.dram_in("w", [K, N])
    y = P.dram_out("y", [T, N])
    ws = P.sbuf("ws", [128, KC, N], BF16); wsB = P.buf("ws")
    for k0 in range(0, KC, 8):
        k1 = min(KC, k0 + 8)
        P.dma("pool", ws[:, k0:k1, :], w[k0 * 128:k1 * 128, :].rearrange("(k p) n -> p k n", p=128), [], [wsB], wsB)
    xt = P.sbuf("xt", [128, K], BF16); xB = P.buf("xt")
    xT = P.sbuf("xT", [128, KC, 128], BF16); xTB = P.buf("xT")
    yt = P.sbuf("yt", [128, N], F32); ytB = P.buf("yt")
    pbf = P.psum("pbf", [128, 512], BF16); pbfB = P.buf()
    pp = [P.psum("pp%d" % i, [128, 512], F32) for i in range(2)]
    ppB = [P.buf() for _ in range(2)]
    pi = 0
    NSW = min(512, N)
    for ti in range(T // 128):
        r0 = ti * 128
        P.dma("sp", xt[:], x[r0:r0 + 128, :], [], [xB], xB)
        for g4 in range(0, KC, 4):
            n4 = min(4, KC - g4)
            for q in range(n4):
                P.tr(pbf[:, q * 128:(q + 1) * 128], xt[:, (g4 + q) * 128:(g4 + q + 1) * 128], P.identb[:], [xB, P.identB], [pbfB])
            cp(P, "act", xT[:, g4:g4 + n4, :], pbf[:, 0:n4 * 128].rearrange("p (q t) -> p q t", q=n4), [pbfB], [xTB])
        for ns in range(0, N, NSW):
            p = pi % 2; pi += 1
            for k in range(KC):
                P.mm(pp[p][:, 0:NSW], xT[:, k, :], ws[:, k, ns:ns + NSW], k == 0, k == KC - 1, [xTB, wsB], [ppB[p]])
            cp(P, "act", yt[:, ns:ns + NSW], pp[p][:, 0:NSW], [ppB[p]], [ytB])
        P.dma("sp", y[r0:r0 + 128, :], yt[:], [ytB], [], ytB)
    return P


def arrange_win(w_in, QH, VH, qheads):
    QK = QH * 128; V = VH * 128
    outs = []
    for Hq in qheads:
        cols = [np.arange(Hq * 128, (Hq + 1) * 128), QK + np.arange(Hq * 128, (Hq + 1) * 128)]
        for vh in range(2):
            Hv = 2 * Hq + vh
            cols.append(2 * QK + np.arange(Hv * 128, (Hv + 1) * 128))
            cols.append(2 * QK + V + np.arange(Hv * 128, (Hv + 1) * 128))
            ab0 = 2 * QK + 2 * V
            cols.append(np.array([ab0 + 0 * 2 * VH + 0 * VH + Hv, ab0 + 1 * 2 * VH + 0 * VH + Hv,
                                  ab0 + 0 * 2 * VH + 1 * VH + Hv, ab0 + 1 * 2 * VH + 1 * VH + Hv]))
        outs.append(w_in[:, np.concatenate(cols)])
    return np.ascontiguousarray(np.stack(outs))

def arrange_cw(conv_w, QH, VH, qheads):
    QK = QH * 128
    outs = []
    for Hq in qheads:
        chunks = [conv_w[:, Hq * 128:(Hq + 1) * 128], conv_w[:, QK + Hq * 128: QK + (Hq + 1) * 128]]
        for vh in range(2):
            Hv = 2 * Hq + vh
            chunks.append(conv_w[:, 2 * QK + Hv * 128: 2 * QK + (Hv + 1) * 128])
        outs.append(np.stack([c.T for c in chunks], axis=1))
    return np.ascontiguousarray(np.stack(outs))

def arrange_hp(a_log, dt_bias, qheads):
    outs = []
    for Hq in qheads:
        row = []
        for vh in range(2):
            Hv = 2 * Hq + vh
            row += [a_log[0, Hv], a_log[1, Hv], dt_bias[0, Hv], dt_bias[1, Hv]]
        outs.append(np.broadcast_to(np.array(row, np.float32), (128, 8)))
    return np.ascontiguousarray(np.stack(outs))


D_MODEL = 2048
NB = 4
SEQ = 4096
LCTX = 256
GRID_W = 64
NCORE = 8
TPC = 2176
TALL = NCORE * TPC
CONF_SUBS = [(640, 512, 1)] * 4 + [(256, 128, 0)]

_PROGS = {}


def _prog(name):
    if name in _PROGS:
        return _PROGS[name]
    t = _time.time()
    D = D_MODEL
    if name == "mod":
        P = build_mod(D, 1536, 4)
    elif name == "dn":
        P = build_dn(D, LCTX + SEQ, LCTX, 8, True, D, outproj=False)
    elif name == "lin":
        P = build_lin(TPC, 4096, D)
    elif name == "conf":
        P = build_conf(D, CONF_SUBS, True)
    elif name == "mid":
        P = build_mid(TPC, D, True, True, False, False, [(0, 16), (16, 17)])
    elif name == "final":
        P = build_mid(TPC, D, True, False, False, True, [(0, 16), (16, 17)])
    elif name == "moe":
        P = build_moe(TALL, D, 384, 8)
    else:
        raise KeyError(name)
    nc = P.emit()
    _PROGS[name] = nc
    print("[kernel] built %s %s in %.1fs" % (name, P.ninstr(), _time.time() - t), flush=True)
    return nc


def _launch(name, in_maps):
    nc = _prog(name)
    t = _time.time()
    res = run_bass_kernel_spmd(nc, in_maps, core_ids=list(range(len(in_maps)))).results
    print("[kernel] ran %s in %.1fs" % (name, _time.time() - t), flush=True)
    return res


def _bc(v, rows=128):
    v = np.asarray(v, np.float32)
    return np.ascontiguousarray(np.broadcast_to(v, (rows, v.shape[-1])))


def _mkvec(segs):
    out = np.zeros((len(segs), 5, 128, D_MODEL), np.float32)
    for si, vs_ in enumerate(segs):
        for k, v in enumerate(vs_):
            if v is not None:
                out[si, k] = np.asarray(v, np.float32)[None, :]
    return out


def _to_cm(h):
    b, n, d = h.shape
    r = n // GRID_W
    return np.ascontiguousarray(h.reshape(b, r, GRID_W, d).transpose(0, 2, 1, 3).reshape(b, n, d))


def _from_cm(h):
    b, n, d = h.shape
    r = n // GRID_W
    return np.ascontiguousarray(h.reshape(b, GRID_W, r, d).transpose(0, 2, 1, 3).reshape(b, n, d))


def _halo(seq, start, TS, TV):
    L, d = seq.shape
    out = np.zeros((TS, d), np.float32)
    m = np.zeros((TS,), np.float32)
    lo = start - HALO
    j0 = max(0, -lo)
    j1 = min(TV + 2 * HALO, L - lo)
    out[j0:j1] = seq[lo + j0:lo + j1]
    m[j0:j1] = 1.0
    return out, m


def kernel(**inp):
    D = D_MODEL
    g = lambda k: np.asarray(inp[k])
    f32 = lambda a: np.ascontiguousarray(np.asarray(a, dtype=np.float32))
    x, c, ctx, c_ctx = f32(g("x")), f32(g("c")), f32(g("ctx")), f32(g("c_ctx"))
    t_all = _time.time()
    cc = np.concatenate([c, c_ctx[None]], 0)
    ccT = np.ascontiguousarray(cc.T.reshape(D // 128, 128, 5).transpose(1, 0, 2))
    ada_w, ada_b = g("ada_w"), g("ada_b")
    NCO = 1536
    ims = [dict(ccT=ccT, aw=f32(ada_w[:, :, q * NCO:(q + 1) * NCO]),
                ab=np.ascontiguousarray(np.broadcast_to(ada_b[:, None, q * NCO:(q + 1) * NCO], (4, 5, NCO)).astype(np.float32)))
           for q in range(NCORE)]
    res = _launch("mod", ims)
    mod = np.concatenate([r["mod"] for r in res], axis=2)
    mv = lambda i, row, k: mod[i, row, k * D:(k + 1) * D]

    XL, XC = x, ctx
    YL, YC = np.zeros_like(XL), np.zeros_like(XC)
    zD = np.zeros((D,), np.float32)
    gtp_l, gtp_c = [zD] * NB, zD
    for i in range(4):
        j = i // 2
        colm = (i // 2) % 2 == 1
        XLt = _to_cm(XL) if colm else XL
        YLt = _to_cm(YL) if colm else YL
        n1g, n2g = g("norm1_g")[i], g("norm2_g")[i]
        YALt = np.zeros_like(XL)
        YAC = np.zeros_like(XC)
        if i % 2 == 0:
            ims = []
            w_in, conv_w = g("dn_w_in")[j], g("dn_conv_w")[j]
            a_log, dtb, ong = g("dn_a_log")[j], g("dn_dt_bias")[j], g("dn_onorm_g")[j]
            arr = {}
            for hh in range(2):
                qh = list(range(hh * 8, hh * 8 + 8))
                arr[hh] = (arrange_win(w_in, 16, 32, qh), arrange_cw(conv_w, 16, 32, qh), arrange_hp(a_log, dtb, qh))
            ogb = _bc(ong)
            for b in range(NB):
                xp = np.concatenate([XC[b], XLt[b]], 0)
                yp = np.concatenate([YC[b], YLt[b]], 0)
                vec = _mkvec([[gtp_c, None, n1g, mv(i, 4, 1), mv(i, 4, 0)], [gtp_l[b], None, n1g, mv(i, b, 1), mv(i, b, 0)]])
                for hh in range(2):
                    ims.append(dict(xp=xp, yp=yp, vec=vec, win=arr[hh][0], cw=arr[hh][1], hp=arr[hh][2], og=ogb))
            res = _launch("dn", ims)
            w_out = f32(g("dn_w_out")[j])
            ims = []
            for b in range(NB):
                ogf = np.concatenate([res[2 * b]["og_out"], res[2 * b + 1]["og_out"]], axis=1)
                for s in range(2):
                    ims.append(dict(x=np.ascontiguousarray(ogf[s * TPC:(s + 1) * TPC]), w=w_out))
            res = _launch("lin", ims)
            for b in range(NB):
                yt = np.concatenate([res[2 * b]["y"], res[2 * b + 1]["y"]], 0)
                YAC[b] = yt[:LCTX]
                YALt[b] = yt[LCTX:]
        else:
            w1a, cb = arrange_conf_w(g("cv_w1")[j], g("cv_b1")[j], g("cv_dw")[j], g("cv_dwb")[j], g("cv_ln_g")[j], g("cv_ln_b")[j])
            w2, b2 = f32(g("cv_w2")[j]), _bc(g("cv_b2")[j])
            ims = []
            for b in range(NB):
                vec = _mkvec([[gtp_c, None, n1g, mv(i, 4, 1), mv(i, 4, 0)], [gtp_l[b], None, n1g, mv(i, b, 1), mv(i, b, 0)]])
                for s in range(2):
                    xs, ys_, ms = [], [], []
                    for q in range(4):
                        st0 = s * 2048 + q * 512
                        a, m = _halo(XLt[b], st0, 640, 512)
                        bb, _ = _halo(YLt[b], st0, 640, 512)
                        xs.append(a); ys_.append(bb); ms.append(m)
                    a, m = _halo(XC[b], s * 128, 256, 128)
                    bb, _ = _halo(YC[b], s * 128, 256, 128)
                    xs.append(a); ys_.append(bb); ms.append(m)
                    mk = np.concatenate(ms)
                    ims.append(dict(xp=np.concatenate(xs, 0), yp=np.concatenate(ys_, 0), vec=vec,
                                    mask=np.ascontiguousarray(np.broadcast_to(mk, (128, mk.shape[0]))),
                                    w1=w1a, cb=cb, w2=w2, b2=b2))
            res = _launch("conf", ims)
            for b in range(NB):
                for s in range(2):
                    yv = res[2 * b + s]["y"]
                    YALt[b, s * 2048:(s + 1) * 2048] = yv[:2048]
                    YAC[b, s * 128:(s + 1) * 128] = yv[2048:2176]
        YAL = _from_cm(YALt) if colm else YALt
        wr = f32(np.concatenate([g("moe_w_grp")[i], g("moe_w_exp")[i]], 1))
        br = _bc(np.concatenate([g("moe_b_grp")[i], g("moe_b_exp")[i]]))
        ims = []
        for b in range(NB):
            vec = _mkvec([[gtp_l[b], mv(i, b, 2), n2g, mv(i, b, 4), mv(i, b, 3)], [gtp_c, mv(i, 4, 2), n2g, mv(i, 4, 4), mv(i, 4, 3)]])
            for s in range(2):
                sl, cs_ = slice(s * 2048, (s + 1) * 2048), slice(s * 128, (s + 1) * 128)
                ims.append(dict(xp=np.concatenate([XL[b, sl], XC[b, cs_]], 0), yp=np.concatenate([YL[b, sl], YC[b, cs_]], 0),
                                ya=np.concatenate([YAL[b, sl], YAC[b, cs_]], 0), vec=vec, wr=wr, br=br))
        res = _launch("mid", ims)
        X1L, X1C = np.empty_like(XL), np.empty_like(XC)
        for b in range(NB):
            for s in range(2):
                x1 = res[2 * b + s]["x1"]
                X1L[b, s * 2048:(s + 1) * 2048] = x1[:2048]
                X1C[b, s * 128:(s + 1) * 128] = x1[2048:]
        hT_all = np.ascontiguousarray(np.concatenate([r["hT"] for r in res], axis=2))
        gates_all = np.concatenate([r["gates"] for r in res], 0)
        gsel_all = np.concatenate([r["gsel"] for r in res], 0)
        del res
        wgu, wdn = g("moe_w_gu")[i], g("moe_w_down")[i]
        ims = [dict(hT=hT_all, gates=np.ascontiguousarray(gates_all[:, q * 8:(q + 1) * 8]),
                    wgu=f32(wgu[q * 8:(q + 1) * 8]), wdn=f32(wdn[q * 8:(q + 1) * 8])) for q in range(NCORE)]
        res = _launch("moe", ims)
        grp = np.argmax(gsel_all, axis=1)
        ym = np.empty((TALL, D), np.float32)
        for q in range(NCORE):
            m = grp == q
            ym[m] = res[q]["y"][m]
        del res
        YL, YC = np.empty_like(XL), np.empty_like(XC)
        for b in range(NB):
            for s in range(2):
                base = (2 * b + s) * TPC
                YL[b, s * 2048:(s + 1) * 2048] = ym[base:base + 2048]
                YC[b, s * 128:(s + 1) * 128] = ym[base + 2048:base + TPC]
        XL, XC = X1L, X1C
        gtp_l, gtp_c = [mv(i, b, 5) for b in range(NB)], mv(i, 4, 5)
        print("[kernel] layer %d done at %.0fs" % (i, _time.time() - t_all), flush=True)
    fg = g("final_g")
    ims = []
    for b in range(NB):
        vec = _mkvec([[gtp_l[b], None, fg, None, None], [gtp_c, None, fg, None, None]])
        for s in range(2):
            sl, cs_ = slice(s * 2048, (s + 1) * 2048), slice(s * 128, (s + 1) * 128)
            ims.append(dict(xp=np.concatenate([XL[b, sl], XC[b, cs_]], 0), yp=np.concatenate([YL[b, sl], YC[b, cs_]], 0), vec=vec))
    res = _launch("final", ims)
    out = np.empty((NB, SEQ, D), np.float32)
    for b in range(NB):
        for s in range(2):
            out[b, s * 2048:(s + 1) * 2048] = res[2 * b + s]["out"][:2048]
    print("[kernel] total %.0fs" % (_time.time() - t_all), flush=True)
    return out
```
